# Optimizing a Trainium2 kernel written in Bass

```python
import math
import jax, jax.numpy as jnp
from jax import lax
import numpy as np

D_MODEL = 2048
BATCH = 4
SEQ = 8192
DEPTH = 1

MIX_WIDTH = D_MODEL
RWKV_HEAD_DIM = 64
RWKV_WIDTH = MIX_WIDTH // 2
RWKV_HEADS = RWKV_WIDTH // RWKV_HEAD_DIM
RWKV_DECAY_LORA = 64
RWKV_ICLR_LORA = 64
RWKV_GATE_LORA = 160
RWKV_GN_EPS = 64e-5
SSM_HEAD_DIM = 64
SSM_WIDTH = MIX_WIDTH - RWKV_WIDTH
SSM_HEADS = SSM_WIDTH // SSM_HEAD_DIM
SSM_GROUPS = 2
SSM_HEADS_PER_GROUP = SSM_HEADS // SSM_GROUPS
SSM_STATE = 128
SSM_CONV = 4
SSM_CHUNK = 128
SSM_CONV_CH = SSM_WIDTH + 2 * SSM_GROUPS * SSM_STATE
SSM_NORM_EPS = 1e-5
RWKV_PROJ = 3 * RWKV_WIDTH + RWKV_DECAY_LORA + RWKV_ICLR_LORA + RWKV_GATE_LORA
SSM_PROJ = SSM_WIDTH + SSM_CONV_CH + SSM_HEADS
IN_PROJ = RWKV_PROJ + SSM_PROJ
PEER_HEADS = 8
PEER_N_KEYS = 128
PEER_EXPERTS = PEER_N_KEYS * PEER_N_KEYS
PEER_KEY_DIM = 256
PEER_HALF = PEER_KEY_DIM // 2
PEER_TOPK = 16
PEER_BLOCK = 128
RMS_EPS = 1e-6

kernel_name = "hymba_rwkv7_mamba2_peer_adaln"


def rms_norm(x, w, eps=RMS_EPS):
    xf = x.astype(jnp.float32)
    return xf * lax.rsqrt(jnp.mean(xf * xf, axis=-1, keepdims=True) + eps) * w.astype(jnp.float32)


def modulate(x, shift, scale):
    return x * (1.0 + scale[:, None, :]) + shift[:, None, :]


def token_shift(p):
    return jnp.pad(p, ((0, 0), (1, 0), (0, 0)))[:, :-1]


def rwkv7_recurrence(r, w, k, v, kk, a):
    bsz, _, h, n = r.shape

    def step(S, inp):
        r_t, w_t, k_t, v_t, kk_t, a_t = inp
        sa = jnp.einsum('bhvk,bhk->bhv', S, kk_t)
        S = (S * w_t[:, :, None, :]
             - sa[..., None] * (kk_t * a_t)[:, :, None, :]
             + v_t[..., None] * k_t[:, :, None, :])
        return S, jnp.einsum('bhvk,bhk->bhv', S, r_t)

    S0 = jnp.zeros((bsz, h, n, n), jnp.float32)
    xs = (jnp.moveaxis(r, 1, 0), jnp.moveaxis(w, 1, 0), jnp.moveaxis(k, 1, 0),
          jnp.moveaxis(v, 1, 0), jnp.moveaxis(kk, 1, 0), jnp.moveaxis(a, 1, 0))
    _, o = lax.scan(step, S0, xs)
    return jnp.moveaxis(o, 0, 1)


def rwkv7_mix(p, mu, w0, w_up, a0, a_up, g_up, k_k, k_a, r_k, ln_w, ln_b):
    bsz, t, _ = p.shape
    p = p + (token_shift(p) - p) * mu
    s0 = RWKV_WIDTH
    r, k, v, wd, ad, gd = jnp.split(
        p, [s0, 2 * s0, 3 * s0, 3 * s0 + RWKV_DECAY_LORA,
            3 * s0 + RWKV_DECAY_LORA + RWKV_ICLR_LORA], axis=-1)
    w_log = -jax.nn.softplus(-(w0 + jnp.tanh(wd) @ w_up)) - 0.5
    decay = jnp.exp(-jnp.exp(w_log))
    a = jax.nn.sigmoid(a0 + ad @ a_up)
    g = jax.nn.sigmoid(gd) @ g_up
    heads = lambda z: z.reshape(bsz, t, RWKV_HEADS, RWKV_HEAD_DIM)
    kk = heads(k * k_k)
    kk = kk / jnp.maximum(jnp.sqrt(jnp.sum(kk * kk, axis=-1, keepdims=True)), 1e-12)
    k = k * (1.0 + (a - 1.0) * k_a)
    r, k, v, decay, a = heads(r), heads(k), heads(v), heads(decay), heads(a)
    o = rwkv7_recurrence(r, decay, k, v, kk, a)
    mean = jnp.mean(o, axis=-1, keepdims=True)
    var = jnp.mean(jnp.square(o - mean), axis=-1, keepdims=True)
    o = ((o - mean) * lax.rsqrt(var + RWKV_GN_EPS)).reshape(bsz, t, RWKV_WIDTH) * ln_w + ln_b
    bonus = jnp.sum(r * k * r_k, axis=-1, keepdims=True) * v
    return (o + bonus.reshape(bsz, t, RWKV_WIDTH)) * g


def segsum_exp(a):
    n = a.shape[-1]
    cs = jnp.cumsum(a, axis=-1)
    diff = cs[..., :, None] - cs[..., None, :]
    mask = jnp.tril(jnp.ones((n, n), dtype=bool))
    return jnp.exp(jnp.where(mask, diff, -jnp.inf))


def ssd_chunked(x, a, b, c):
    bsz, t, g, r, p = x.shape
    n = b.shape[-1]
    nc, L = t // SSM_CHUNK, SSM_CHUNK
    x = x.reshape(bsz, nc, L, g, r, p)
    b = b.reshape(bsz, nc, L, g, n)
    c = c.reshape(bsz, nc, L, g, n)
    a = a.reshape(bsz, nc, L, g, r).transpose(0, 3, 4, 1, 2)
    a_cs = jnp.cumsum(a, axis=-1)
    cb = jnp.einsum('bclgn,bcsgn->bgcls', c, b)
    y_diag = jnp.einsum('bgrcls,bcsgrp->bclgrp', cb[:, :, None] * segsum_exp(a), x)
    decay_to_end = jnp.exp(a_cs[..., -1:] - a_cs)
    states = jnp.einsum('bclgn,bgrcl,bclgrp->bcgrpn', b, decay_to_end, x)
    states = jnp.pad(states, ((0, 0), (1, 0), (0, 0), (0, 0), (0, 0), (0, 0)))
    decay_chunk = segsum_exp(jnp.pad(a_cs[..., -1], ((0, 0), (0, 0), (0, 0), (1, 0))))
    states = jnp.einsum('bgrzc,bcgrpn->bzgrpn', decay_chunk, states)[:, :-1]
    y_off = jnp.einsum('bclgn,bcgrpn,bgrcl->bclgrp', c, states, jnp.exp(a_cs))
    return (y_diag + y_off).reshape(bsz, t, g, r, p)


def mamba2_mix(p, conv_w, conv_b, dt_bias, a_log, d_skip, norm_w):
    bsz, t, _ = p.shape
    z, xbc, dt = jnp.split(p, [SSM_WIDTH, SSM_WIDTH + SSM_CONV_CH], axis=-1)
    xbc = lax.conv_general_dilated(
        xbc, conv_w[:, None, :].astype(xbc.dtype), window_strides=(1,),
        padding=[(SSM_CONV - 1, 0)], dimension_numbers=('NWC', 'WIO', 'NWC'),
        feature_group_count=SSM_CONV_CH) + conv_b
    xbc = jax.nn.silu(xbc)
    xs, bm, cm = jnp.split(xbc, [SSM_WIDTH, SSM_WIDTH + SSM_GROUPS * SSM_STATE], axis=-1)
    dt = jax.nn.softplus(dt + dt_bias)
    A = -jnp.exp(a_log.astype(jnp.float32))
    G, R = SSM_GROUPS, SSM_HEADS_PER_GROUP
    xs = xs.reshape(bsz, t, G, R, SSM_HEAD_DIM)
    dt_g = dt.reshape(bsz, t, G, R)
    y = ssd_chunked(xs * dt_g[..., None], dt_g * A.reshape(G, R),
                    bm.reshape(bsz, t, G, SSM_STATE), cm.reshape(bsz, t, G, SSM_STATE))
    y = y + xs * d_skip.reshape(G, R)[..., None]
    y = y.reshape(bsz, t, SSM_WIDTH) * jax.nn.silu(z)
    y = y.reshape(bsz, t, G, SSM_WIDTH // G)
    y = y * lax.rsqrt(jnp.mean(y * y, axis=-1, keepdims=True) + SSM_NORM_EPS)
    return y.reshape(bsz, t, SSM_WIDTH) * norm_w


def peer_ffn(n, w_query, sub_keys, u_emb, v_emb):
    bsz, t, d = n.shape
    blocks = n.reshape(bsz * t // PEER_BLOCK, PEER_BLOCK, d)

    def one_block(xb):
        q = (xb @ w_query).reshape(PEER_BLOCK, PEER_HEADS, 2, PEER_HALF)
        s = jnp.einsum('lhpd,hpnd->lhpn', q, sub_keys).astype(jnp.float32)
        s_half, i_half = lax.top_k(s, PEER_TOPK)
        cand_s = (s_half[:, :, 0, :, None] + s_half[:, :, 1, None, :]).reshape(
            PEER_BLOCK, PEER_HEADS, PEER_TOPK * PEER_TOPK)
        cand_i = (i_half[:, :, 0, :, None] * PEER_N_KEYS + i_half[:, :, 1, None, :]).reshape(
            PEER_BLOCK, PEER_HEADS, PEER_TOPK * PEER_TOPK)
        top_s, pos = lax.top_k(cand_s, PEER_TOPK)
        eidx = jnp.take_along_axis(cand_i, pos, axis=-1)
        gates = jax.nn.softmax(top_s, axis=-1)
        u = u_emb[eidx]
        act = jax.nn.gelu(jnp.einsum('ld,lhkd->lhk', xb, u), approximate=False)
        return jnp.einsum('lhk,lhkd->ld', gates * act, v_emb[eidx])

    return lax.map(one_block, blocks).reshape(bsz, t, d)


def setup_inputs(seed: int = 0) -> dict:
    key = jax.random.key(seed)
    ks = jax.random.split(key, 32)
    nrm = lambda i, shape, s: jax.random.normal(ks[i], shape, jnp.float32) * s
    L = DEPTH
    dt0 = jnp.exp(jax.random.uniform(ks[17], (L, SSM_HEADS), jnp.float32,
                                     minval=math.log(1e-3), maxval=math.log(1e-1)))
    return {
        "x": nrm(0, (BATCH, SEQ, D_MODEL), 1.0),
        "c": nrm(1, (BATCH, D_MODEL), 1.0),
        "ada_w": nrm(2, (L, D_MODEL, 6 * D_MODEL), 0.5 * D_MODEL ** -0.5),
        "ada_b": nrm(3, (L, 6 * D_MODEL), 0.02),
        "norm1_w": 1.0 + nrm(4, (L, D_MODEL), 0.02),
        "w_in": nrm(5, (L, D_MODEL, IN_PROJ), D_MODEL ** -0.5),
        "rwkv_mu": jax.random.uniform(ks[6], (L, RWKV_PROJ), jnp.float32),
        "rwkv_w0": jax.random.uniform(ks[7], (L, RWKV_WIDTH), jnp.float32, minval=-6.5, maxval=-1.5),
        "rwkv_w_up": nrm(8, (L, RWKV_DECAY_LORA, RWKV_WIDTH), 0.5 * RWKV_DECAY_LORA ** -0.5),
        "rwkv_a0": nrm(9, (L, RWKV_WIDTH), 0.1),
        "rwkv_a_up": nrm(10, (L, RWKV_ICLR_LORA, RWKV_WIDTH), 0.5 * RWKV_ICLR_LORA ** -0.5),
        "rwkv_g_up": nrm(11, (L, RWKV_GATE_LORA, RWKV_WIDTH), RWKV_GATE_LORA ** -0.5),
        "rwkv_k_k": 0.85 + nrm(12, (L, RWKV_WIDTH), 0.02),
        "rwkv_k_a": 1.0 + nrm(13, (L, RWKV_WIDTH), 0.02),
        "rwkv_r_k": nrm(14, (L, RWKV_HEADS, RWKV_HEAD_DIM), 0.1),
        "rwkv_ln_w": 1.0 + nrm(15, (L, RWKV_WIDTH), 0.02),
        "rwkv_ln_b": nrm(16, (L, RWKV_WIDTH), 0.02),
        "mamba_conv_w": nrm(18, (L, SSM_CONV, SSM_CONV_CH), SSM_CONV ** -0.5),
        "mamba_conv_b": nrm(19, (L, SSM_CONV_CH), 0.02),
        "mamba_dt_bias": dt0 + jnp.log(-jnp.expm1(-dt0)),
        "mamba_a_log": jnp.log(jax.random.uniform(ks[20], (L, SSM_HEADS), jnp.float32, minval=1.0, maxval=16.0)),
        "mamba_d": 1.0 + nrm(21, (L, SSM_HEADS), 0.02),
        "mamba_norm_w": 1.0 + nrm(22, (L, SSM_WIDTH), 0.02),
        "w_out": nrm(23, (L, MIX_WIDTH, D_MODEL), MIX_WIDTH ** -0.5),
        "norm2_w": 1.0 + nrm(24, (L, D_MODEL), 0.02),
        "peer_w_query": nrm(25, (L, D_MODEL, PEER_HEADS * PEER_KEY_DIM), D_MODEL ** -0.5),
        "peer_sub_keys": nrm(26, (L, PEER_HEADS, 2, PEER_N_KEYS, PEER_HALF), PEER_HALF ** -0.5),
        "peer_u": nrm(27, (L, PEER_EXPERTS, D_MODEL), D_MODEL ** -0.5),
        "peer_v": nrm(28, (L, PEER_EXPERTS, D_MODEL), 1.0),
        "final_norm_w": 1.0 + nrm(29, (D_MODEL,), 0.02),
    }


def reference(x, c, ada_w, ada_b, norm1_w, w_in, rwkv_mu, rwkv_w0, rwkv_w_up, rwkv_a0,
              rwkv_a_up, rwkv_g_up, rwkv_k_k, rwkv_k_a, rwkv_r_k, rwkv_ln_w, rwkv_ln_b,
              mamba_conv_w, mamba_conv_b, mamba_dt_bias, mamba_a_log, mamba_d, mamba_norm_w,
              w_out, norm2_w, peer_w_query, peer_sub_keys, peer_u, peer_v, final_norm_w):
    out_dtype = x.dtype
    h = x.astype(jnp.float32)
    cond = jax.nn.silu(c.astype(jnp.float32))
    for i in range(DEPTH):
        mod = cond @ ada_w[i] + ada_b[i]
        shift1, scale1, gate1, shift2, scale2, gate2 = jnp.split(mod, 6, axis=-1)
        n = modulate(rms_norm(h, norm1_w[i]), shift1, scale1)
        proj = n @ w_in[i]
        o_rwkv = rwkv7_mix(proj[..., :RWKV_PROJ], rwkv_mu[i], rwkv_w0[i], rwkv_w_up[i],
                           rwkv_a0[i], rwkv_a_up[i], rwkv_g_up[i], rwkv_k_k[i], rwkv_k_a[i],
                           rwkv_r_k[i], rwkv_ln_w[i], rwkv_ln_b[i])
        o_ssm = mamba2_mix(proj[..., RWKV_PROJ:], mamba_conv_w[i], mamba_conv_b[i],
                           mamba_dt_bias[i], mamba_a_log[i], mamba_d[i], mamba_norm_w[i])
        mix = jnp.concatenate([o_rwkv, o_ssm], axis=-1) @ w_out[i]
        h = h + gate1[:, None, :] * mix
        n = modulate(rms_norm(h, norm2_w[i]), shift2, scale2)
        h = h + gate2[:, None, :] * peer_ffn(n, peer_w_query[i], peer_sub_keys[i], peer_u[i], peer_v[i])
    return rms_norm(h, final_norm_w).astype(out_dtype)
```

```python
from contextlib import ExitStack
import numpy as np
import concourse.bass as bass
import concourse.mybir as mybir
from concourse.bass_utils import run_bass_kernel_spmd

F32 = mybir.dt.float32
BF16 = mybir.dt.bfloat16
U32 = mybir.dt.uint32
ALU = mybir.AluOpType
AF = mybir.ActivationFunctionType
AX = mybir.AxisListType

D = 2048
KD = 16
PAD = 64
C0 = 0.6065306597126334
NEXP = 16384


class P:
    ENG = ['pe', 'dve', 'act', 'pool', 'sp']
    LIM = 30000
    DLIM = 1500

    def __init__(self, nc):
        self.nc = nc
        self.es = ExitStack()
        self.ops = {e: [] for e in self.ENG}
        self.cnt = {e: 0 for e in self.ENG}
        self.epoch = {e: 0 for e in self.ENG}
        self.waited = {e: {} for e in self.ENG}
        self.last_w = {}
        self.readers = {}
        self.dcnt = {}
        self.depoch = {}
        self.semkeys = set()
        self.n_ops = 0

    def sbuf(self, name, shape, dtype):
        return self.es.enter_context(self.nc.sbuf_tensor(name, shape, dtype))

    def psum(self, name, shape, dtype):
        return self.es.enter_context(self.nc.psum_tensor(name, shape, dtype))

    def _deps(self, eng, reads, writes, skip_same=False):
        deps = []
        for k in reads:
            t = self.last_w.get(k)
            if t is not None:
                deps.append(t)
        for k in writes:
            t = self.last_w.get(k)
            if t is not None:
                deps.append(t)
            deps.extend(self.readers.get(k, ()))
        need = {}
        for key, v in deps:
            if skip_same and key[0] == 'e' and key[1] == eng:
                continue
            if need.get(key, 0) < v:
                need[key] = v
        waits = []
        w = self.waited[eng]
        for key, v in need.items():
            if w.get(key, 0) >= v:
                continue
            w[key] = v
            waits.append((key, v))
        return waits

    def _commit(self, tok, reads, writes):
        for k in writes:
            self.last_w[k] = tok
            self.readers[k] = []
        for k in reads:
            if k in writes:
                continue
            r = self.readers.setdefault(k, [])
            r.append(tok)
            if len(r) > 64:
                m = {}
                for key, v in r:
                    if m.get(key, 0) < v:
                        m[key] = v
                self.readers[k] = list(m.items())

    def op(self, eng, fn, reads=(), writes=(), skip_same=False):
        waits = self._deps(eng, reads, writes, skip_same)
        if self.cnt[eng] >= self.LIM:
            self.epoch[eng] += 1
            self.cnt[eng] = 0
        self.cnt[eng] += 1
        key = ('e', eng, self.epoch[eng])
        self.semkeys.add(key)
        tok = (key, self.cnt[eng])
        self.ops[eng].append(('op', waits, fn, key))
        self._commit(tok, reads, writes)
        self.n_ops += 1

    def dma_fn(self, eng, fn, reads=(), writes=(), sem='dma'):
        waits = self._deps(eng, reads, writes)
        if self.dcnt.get(sem, 0) >= self.DLIM:
            self.depoch[sem] = self.depoch.get(sem, 0) + 1
            self.dcnt[sem] = 0
        self.dcnt[sem] = self.dcnt.get(sem, 0) + 1
        key = ('d', sem, self.depoch.get(sem, 0))
        self.semkeys.add(key)
        tok = (key, 16 * self.dcnt[sem])
        self.ops[eng].append(('dma', waits, fn, key))
        self._commit(tok, reads, writes)
        self.n_ops += 1

    def dma(self, eng, out, in_, reads=(), writes=(), sem=None, **kw):
        if sem is None or sem == 'c0':
            self.n_uniq = getattr(self, 'n_uniq', 0) + 1
            sem = 'u%d' % self.n_uniq
        self.dma_fn(eng, lambda e: e.dma_start(out=out, in_=in_, **kw), reads, writes, sem)

    def barrier(self):
        allw = []
        for e in self.ENG:
            if self.cnt[e] > 0:
                allw.append((('e', e, self.epoch[e]), self.cnt[e]))
        for sname, c in self.dcnt.items():
            allw.append((('d', sname, self.depoch.get(sname, 0)), 16 * c))
        for e in self.ENG:
            waits = []
            w = self.waited[e]
            for key, v in allw:
                if w.get(key, 0) >= v:
                    continue
                w[key] = v
                waits.append((key, v))
            self.ops[e].append(('wait', waits, None, None))

    def finish(self, final_keys=()):
        nc = self.nc
        waits = self._deps('sp', final_keys, [])
        self.ops['sp'].append(('wait', waits, None, None))
        es = self.es
        sems = {}
        for i, key in enumerate(sorted(self.semkeys, key=str)):
            sems[key] = es.enter_context(nc.semaphore('s%d' % i))
        block = es.enter_context(nc.Block())

        def emit(engname):
            def body(e):
                for kind, waits, fn, key in self.ops[engname]:
                    for wk, v in waits:
                        e.wait_ge(sems[wk], v)
                    if kind == 'op':
                        fn(e).then_inc(sems[key], 1)
                    elif kind == 'dma':
                        fn(e).then_inc(sems[key], 16)
            return body

        block.tensor(emit('pe'))
        block.vector(emit('dve'))
        block.scalar(emit('act'))
        block.gpsimd(emit('pool'))
        block.sync(emit('sp'))
        es.close()


class H:
    def __init__(self, p):
        self.p = p

    def tt(self, eng, out, in0, in1, op, r, w):
        self.p.op(eng, lambda e: e.tensor_tensor(out=out, in0=in0, in1=in1, op=op), reads=r, writes=w)

    def ts(self, eng, out, in0, s1, op0, r, w, s2=None, op1=None):
        if op1 is None:
            self.p.op(eng, lambda e: e.tensor_scalar(out=out, in0=in0, scalar1=s1, scalar2=None, op0=op0), reads=r, writes=w)
        else:
            self.p.op(eng, lambda e: e.tensor_scalar(out=out, in0=in0, scalar1=s1, scalar2=s2, op0=op0, op1=op1), reads=r, writes=w)

    def stt(self, out, in0, scalar, in1, op0, op1, r, w, accum_out=None):
        self.p.op('dve', lambda e: e.scalar_tensor_tensor(out=out, in0=in0, scalar=scalar, in1=in1, op0=op0, op1=op1, accum_out=accum_out), reads=r, writes=w)

    def act(self, out, in_, func, r, w, bias=None, scale=None, accum_out=None):
        kw = {}
        if bias is not None:
            kw['bias'] = bias
        if scale is not None:
            kw['scale'] = scale
        if accum_out is not None:
            kw['accum_out'] = accum_out
        self.p.op('act', lambda e: e.activation(out=out, in_=in_, func=func, **kw), reads=r, writes=w)

    def cp(self, eng, out, in_, r, w):
        if eng == 'act':
            self.p.op('act', lambda e: e.activation(out=out, in_=in_, func=AF.Copy), reads=r, writes=w)
        else:
            self.p.op(eng, lambda e: e.tensor_copy(out=out, in_=in_), reads=r, writes=w)

    def mm(self, out, lhsT, rhs, r, w, start=True, stop=True, skip_same=False):
        self.p.op('pe', lambda e: e.matmul(out, lhsT=lhsT, rhs=rhs, start=start, stop=stop), reads=r, writes=w, skip_same=skip_same)

    def tr(self, out, in_, ident, r, w):
        self.p.op('pe', lambda e: e.transpose(out=out, in_=in_, identity=ident), reads=r, writes=w)

    def red(self, out, in_, r, w, op=None):
        self.p.op('dve', lambda e: e.tensor_reduce(out=out, in_=in_, axis=AX.X, op=(op or ALU.add)), reads=r, writes=w)

    def recip(self, out, in_, r, w):
        self.p.op('dve', lambda e: e.reciprocal(out=out, in_=in_), reads=r, writes=w)


class Arena:
    def __init__(self, t, n):
        self.t, self.n, self.off = t, n, 0

    def reset(self):
        self.off = 0

    def alloc(self, shape, dt):
        free = 1
        for d_ in shape[1:]:
            free *= d_
        words = free if dt in (F32, U32) else (free + 1) // 2
        words += words % 2
        v = self.t[0:shape[0], self.off:self.off + words]
        self.off += words
        assert self.off <= self.n, "arena overflow %d > %d" % (self.off, self.n)
        if dt != F32:
            v = v.bitcast(dt)
        if v.shape[1] != free:
            v = v[:, 0:free]
        if len(shape) == 3:
            v = v.rearrange("p (a b) -> p a b", a=shape[1])
        elif len(shape) == 4:
            v = v.rearrange("p (a b c) -> p a b c", a=shape[1], b=shape[2])
        return v


def fm_tiles():
    tl = []
    for i in range(8):
        tl.append([(0 + 128 * i, 128, 0)])
    for i in range(8):
        tl.append([(1024 + 128 * i, 128, 0)])
    for i in range(8):
        tl.append([(2048 + 128 * i, 128, 0)])
    tl.append([(3072, 128, 0)])
    tl.append([(3200, 128, 0)])
    tl.append([(3328, 32, 0)])
    for i in range(8):
        tl.append([(4384 + 128 * i, 128, 0)])
    tl.append([(5408, 128, 0)])
    tl.append([(5536, 128, 0)])
    tl.append([(5664, 128, 0)])
    tl.append([(5792, 128, 0)])
    tl.append([(5920, 16, 0)])
    return tl


NFM = 40
A3_LEVEL = 9
B_LEVEL = 9
STAGES = ['a1', 'a2', 'a3', 'b']


def build_nc(T, TB0, dbg=False):
    NT = T // 512
    nc = bass.Bass("TRN2", target_bir_lowering=False)

    def din(name, shape, dt=F32):
        return nc.dram_tensor(name, list(shape), dt, kind="ExternalInput").ap()

    x_seq = din("x_seq", [T, D])
    maskp = din("maskp", [128, T // 128])
    mask16 = din("mask16", [16, T])
    c_b = din("c_b", [128, KD])
    ada_w = din("ada_w", [D, 6 * D])
    ada_b = din("ada_b", [1, 6 * D])
    nw1 = din("nw1", [1, D])
    nw2 = din("nw2", [1, D])
    fnw = din("fnw", [1, D])
    w_fm = din("w_fm", [NFM, 128, KD, 128])
    w_z = din("w_z", [128, KD, 1024])
    mu_fm = din("mu_fm", [128, 27])
    rw_small = din("rw_small", [128, 8, 5])
    lora_w = din("lora_w", [128, 1024])
    g_up0 = din("g_up0", [128, 1024])
    g_up1 = din("g_up1", [32, 1024])
    ln_wb = din("ln_wb", [2, 1024])
    consts = din("consts", [128, 12, 128])
    conv_p = din("conv_p", [128, 12, 5])
    dt_p = din("dt_p", [16, 2])
    selh = din("selh", [16, 16, 128])
    dsk_nw = din("dsk_nw", [2, 1024])
    w_o = din("w_o", [128, KD, D])
    wq_fm = din("wq_fm", [16, 128, KD, 128])
    skT = din("skT", [128, 16, 128])
    zc_in = din("zc_in", [1, 256])
    peer_u = din("peer_u", [NEXP, D])
    peer_v = din("peer_v", [NEXP, D])
    out = nc.dram_tensor("out", [T - TB0, D], F32, kind="ExternalOutput").ap()

    def scr(name, shape, dt=F32):
        kind = "ExternalOutput" if dbg else "Internal"
        return nc.dram_tensor(name, list(shape), dt, kind=kind).ap()

    projT = scr("projT", [NFM, 128, PAD + T])
    zscr = scr("zscr", [T, 1024])
    oT = scr("oT", [D, T], BF16)
    ubf = nc.dram_tensor("ubf", [NEXP, D], BF16, kind="ExternalOutput").ap()
    vbf = nc.dram_tensor("vbf", [NEXP, D], BF16, kind="ExternalOutput").ap()

    p = P(nc)
    S = p.sbuf
    ctr = [0]

    def uid(s):
        ctr[0] += 1
        return "%s%d" % (s, ctr[0])

    cst = S("cst", [128, 12, 128], F32)
    p.dma('sp', cst[:], consts[:, :, :], writes=['cst'], sem='c0')
    IDF = cst[:, 0, :]
    NSU = cst[:, 1, :]
    NU = cst[:, 2, :]
    SU = cst[:, 3, :]
    UU = cst[:, 4, :]
    NSL = cst[:, 5, :]
    BD1 = cst[:, 6, :]
    TRI = cst[:, 7, :]
    MNEG = cst[:, 8, :]
    ONESF = cst[:, 9, :]
    RST = cst[:, 10, :]
    cstb = S("cstb", [128, 12, 128], BF16)
    p.op('dve', lambda e: e.tensor_copy(out=cstb[:], in_=cst[:]), reads=['cst'], writes=['cstb'])
    IDB = cstb[:, 0, :]
    BD1B = cstb[:, 6, :]
    ONESB = cstb[:, 9, :]

    psall = p.psum("psall", [128, 4096], F32)
    PS = [psall[:, i * 512:(i + 1) * 512] for i in range(8)]

    def psk(i):
        return 'ps%d' % i

    modrow = scr("modrow", [6, D])
    BC_OF_CHUNK = {0: 1, 1: 0, 2: 2, 3: 4, 4: 3, 5: 5}
    AR = 50800
    arena_t = S("arena", [128, AR], F32)
    A = Arena(arena_t, AR)
    if True:
        cT = A.alloc([128, KD], F32)
        cS = A.alloc([128, KD], F32)
        awt = [A.alloc([128, KD, 512], F32) for i in range(2)]
        rows = A.alloc([1, 3, D], F32)
        abr = A.alloc([1, 6 * D], F32)
        rowt = [A.alloc([1, 512], F32) for i in range(2)]
        p.dma('sp', cT[:], c_b[:, :], writes=['cT'], sem='c0')
        p.dma('sp', abr[:], ada_b[:, :], writes=['abr'], sem='c0')
        p.dma('sp', rows[:, 1, :], nw1[:, :], writes=['rows1'], sem='c0')
        p.dma('sp', rows[:, 2, :], nw2[:, :], writes=['rows2'], sem='c0')
        p.op('act', lambda e: e.activation(out=cS[:], in_=cT[:], func=AF.Silu), reads=['cT'], writes=['cS'])
        aw_v = ada_w.rearrange("(j p) c -> p j c", p=128)
        for ct in range(24):
            b = ct % 2
            chunk, q = ct // 4, ct % 4
            p.dma('sp' if b == 0 else 'act', awt[b][:], aw_v[:, :, ct * 512:(ct + 1) * 512],
                  writes=['awt%d' % b], sem='aw%d' % b)
            bank = ct % 2
            for j in range(KD):
                p.op('pe', lambda e, j=j, b=b, bank=bank: e.matmul(
                    PS[bank][0:1, :], lhsT=cS[:, j:j + 1], rhs=awt[b][:, j, :], start=(j == 0), stop=(j == KD - 1)),
                    reads=['cS', 'awt%d' % b], writes=[psk(bank)])
            rt = rowt[b]
            p.op('dve', lambda e, rt=rt, bank=bank, ct=ct: e.tensor_tensor(
                out=rt[:], in0=PS[bank][0:1, :], in1=abr[:, ct * 512:(ct + 1) * 512], op=ALU.add),
                reads=[psk(bank), 'abr'], writes=['rowt%d' % b])
            if chunk in (1, 4):
                nwrow = rows[:, 1 if chunk == 1 else 2, q * 512:(q + 1) * 512]
                p.op('dve', lambda e, rt=rt, nwrow=nwrow: e.scalar_tensor_tensor(
                    out=rt[:], in0=rt[:], scalar=1.0, in1=nwrow, op0=ALU.add, op1=ALU.mult),
                    reads=['rowt%d' % b, 'rows1', 'rows2'], writes=['rowt%d' % b])
            bi = BC_OF_CHUNK[chunk]
            p.dma('sp', modrow[bi:bi + 1, q * 512:(q + 1) * 512], rt[:], reads=['rowt%d' % b], writes=['modrow'], sem='mr%d' % b)

    p.barrier()
    A.reset()
    state = dict(psall=psall, A=A, nc=nc, p=p, T=T, TB0=TB0, NT=NT, PS=PS, psk=psk, cst=cst, cstb=cstb)
    tensors = dict(x_seq=x_seq, maskp=maskp, mask16=mask16, w_fm=w_fm, w_z=w_z, mu_fm=mu_fm,
                   rw_small=rw_small, lora_w=lora_w, g_up0=g_up0, g_up1=g_up1, ln_wb=ln_wb,
                   conv_p=conv_p, dt_p=dt_p, selh=selh, dsk_nw=dsk_nw, w_o=w_o, wq_fm=wq_fm,
                   skT=skT, zc_in=zc_in, peer_u=peer_u, peer_v=peer_v, out=out, modrow=modrow, fnw=fnw, ubf=ubf, vbf=vbf,
                   projT=projT, zscr=zscr, oT=oT)
    stage_a1(state, tensors)
    if 'a2' in STAGES:
        p.barrier()
        A.reset()
        stage_a2(state, tensors)
    if 'a3' in STAGES:
        p.barrier()
        A.reset()
        stage_a3(state, tensors)
    if 'b' in STAGES:
        p.barrier()
        A.reset()
        stage_b(state, tensors)
    final = ['out']
    if dbg:
        final += ['projT', 'zscr', 'oT']
    p.finish(final_keys=final)
    return nc


def stage_a1(st, tn):
    nc, p, T, NT, PS, psk, cstb = st['nc'], st['p'], st['T'], st['NT'], st['PS'], st['psk'], st['cstb']
    IDB = cstb[:, 0, :]
    x_seq, maskp, w_fm, w_z, projT, zscr = tn['x_seq'], tn['maskp'], tn['w_fm'], tn['w_z'], tn['projT'], tn['zscr']
    A = st['A']
    if True:
        S = lambda n, s, d: A.alloc(s, d)
        mk = S("mk", [128, T // 128], F32)
        zeros = S("zeros", [128, NFM, PAD], F32)
        xb = [S("xb%d" % i, [128, D], F32) for i in range(2)]
        t1 = [S("t1_%d" % i, [128, D], F32) for i in range(2)]
        nb = [S("nb%d" % i, [128, D], BF16) for i in range(2)]
        nT = [S("nT%d" % i, [128, KD, 512], BF16) for i in range(2)]
        wt = [S("wt%d" % i, [128, KD, 128], BF16) for i in range(3)]
        wz = S("wz", [128, KD, 1024], BF16)
        stg = [S("stg%d" % i, [128, 512], F32) for i in range(4)]
        sm = [S("sm%d" % i, [128, 4], F32) for i in range(2)]
        bc0 = S("bc0", [128, D], F32)
        bc1 = S("bc1", [128, D], F32)
        p.dma('sp', bc0, tn['modrow'][0:1, :].to_broadcast([128, D]), reads=['modrow'], writes=['bc0'])
        p.dma('act', bc1, tn['modrow'][1:2, :].to_broadcast([128, D]), reads=['modrow'], writes=['bc1'])
        p.dma('sp', mk[:], maskp[:, :], writes=['mk'], sem='c0')
        p.op('pool', lambda e: e.memset(zeros[:], 0.0), writes=['zeros'])
        p.dma('sp', projT[:, :, 0:PAD].rearrange("n p c -> p n c"), zeros[:], reads=['zeros'], writes=['projT'], sem='c0')
        p.dma('pool', wz[:], w_z[:, :, :], writes=['wz'], sem='c0')
        nstg = 0
        nwt = 0
        for it in range(NT):
            ntb = it % 2
            for bl in range(4):
                blk = it * 4 + bl
                b = blk % 2
                p.dma('sp', xb[b][:], x_seq[blk * 128:(blk + 1) * 128, :], writes=['xb%d' % b], sem='xb%d' % b)
                smb = sm[b]
                p.op('act', lambda e, b=b, smb=smb: e.activation(out=t1[b][:], in_=xb[b][:], func=AF.Square, accum_out=smb[:, 0:1]),
                     reads=['xb%d' % b], writes=['t1_%d' % b, 'sm%d' % b])
                p.op('act', lambda e, smb=smb: e.activation(out=smb[:, 1:2], in_=smb[:, 0:1], func=AF.Sqrt, scale=1.0 / D, bias=1e-6),
                     reads=['sm%d' % b], writes=['sm%d' % b])
                p.op('dve', lambda e, smb=smb: e.reciprocal(out=smb[:, 2:3], in_=smb[:, 1:2]), reads=['sm%d' % b], writes=['sm%d' % b])
                p.op('dve', lambda e, b=b, smb=smb: e.scalar_tensor_tensor(
                    out=t1[b][:], in0=xb[b][:], scalar=smb[:, 2:3], in1=bc0, op0=ALU.mult, op1=ALU.mult),
                    reads=['xb%d' % b, 'sm%d' % b, 'bc0'], writes=['t1_%d' % b])
                p.op('pool', lambda e, b=b: e.tensor_tensor(out=t1[b][:], in0=t1[b][:], in1=bc1, op=ALU.add),
                     reads=['t1_%d' % b, 'bc1'], writes=['t1_%d' % b])
                p.op('act', lambda e, b=b, blk=blk: e.activation(out=nb[b][:], in_=t1[b][:], func=AF.Copy, scale=mk[:, blk:blk + 1]),
                     reads=['t1_%d' % b, 'mk'], writes=['nb%d' % b])
                for q in range(4):
                    bank = 4 + (q % 2)
                    pst = PS[bank].bitcast(BF16)
                    for jj in range(4):
                        j = q * 4 + jj
                        p.op('pe', lambda e, pst=pst, jj=jj, j=j, b=b: e.transpose(
                            out=pst[:, jj * 128:(jj + 1) * 128], in_=nb[b][:, j * 128:(j + 1) * 128], identity=IDB),
                            reads=['nb%d' % b, 'cstb'], writes=[psk(bank)])
                    eng = 'dve' if q % 2 == 0 else 'act'
                    dst = nT[ntb][:, q * 4:(q + 1) * 4, bl * 128:(bl + 1) * 128]
                    src = pst[:, 0:512].rearrange("p (a c) -> p a c", a=4)
                    if eng == 'dve':
                        p.op('dve', lambda e, dst=dst, src=src: e.tensor_copy(out=dst, in_=src),
                             reads=[psk(bank)], writes=['nT%d' % ntb])
                    else:
                        p.op('act', lambda e, dst=dst, src=src: e.activation(out=dst, in_=src, func=AF.Copy),
                             reads=[psk(bank)], writes=['nT%d' % ntb])
            for ot in range(NFM):
                wb = nwt % 3
                nwt += 1
                p.dma('pool', wt[wb][:], w_fm[ot, :, :, :], writes=['wt%d' % wb], sem='wt%d' % wb)
                bank = ot % 4
                for j in range(KD):
                    p.op('pe', lambda e, bank=bank, wb=wb, j=j, ntb=ntb: e.matmul(
                        PS[bank][:, :], lhsT=wt[wb][:, j, :], rhs=nT[ntb][:, j, :], start=(j == 0), stop=(j == KD - 1)),
                        reads=['wt%d' % wb, 'nT%d' % ntb], writes=[psk(bank)])
                sb = nstg % 4
                nstg += 1
                if ot % 2 == 0:
                    p.op('dve', lambda e, sb=sb, bank=bank: e.tensor_copy(out=stg[sb][:], in_=PS[bank][:, :]),
                         reads=[psk(bank)], writes=['stg%d' % sb])
                else:
                    p.op('act', lambda e, sb=sb, bank=bank: e.activation(out=stg[sb][:], in_=PS[bank][:, :], func=AF.Copy),
                         reads=[psk(bank)], writes=['stg%d' % sb])
                p.dma('sp', projT[ot, :, PAD + it * 512:PAD + (it + 1) * 512], stg[sb][:],
                      reads=['stg%d' % sb], writes=['projT'], sem='stg%d' % sb)
            for bl in range(4):
                for hf in range(2):
                    bank = 6 + hf
                    for j in range(KD):
                        p.op('pe', lambda e, bank=bank, j=j, ntb=ntb, bl=bl, hf=hf: e.matmul(
                            PS[bank][:, :], lhsT=nT[ntb][:, j, bl * 128:(bl + 1) * 128], rhs=wz[:, j, hf * 512:(hf + 1) * 512],
                            start=(j == 0), stop=(j == KD - 1)),
                            reads=['wz', 'nT%d' % ntb], writes=[psk(bank)])
                    sb = nstg % 4
                    nstg += 1
                    p.op('dve' if hf == 0 else 'act',
                         (lambda e, sb=sb, bank=bank: e.tensor_copy(out=stg[sb][:], in_=PS[bank][:, :])) if hf == 0 else
                         (lambda e, sb=sb, bank=bank: e.activation(out=stg[sb][:], in_=PS[bank][:, :], func=AF.Copy)),
                         reads=[psk(bank)], writes=['stg%d' % sb])
                    r0 = it * 512 + bl * 128
                    p.dma('act', zscr[r0:r0 + 128, hf * 512:(hf + 1) * 512], stg[sb][:],
                          reads=['stg%d' % sb], writes=['zscr'], sem='stg%d' % sb)


def host_consts():
    c = np.zeros((128, 12, 128), np.float32)
    r = np.arange(128)[:, None]
    q = np.arange(128)[None, :]
    same = (r // 64) == (q // 64)
    c[:, 0] = (r == q)
    c[:, 1] = -1.0 * ((r < q) & same)
    c[:, 2] = -1.0 * ((r <= q) & same)
    c[:, 3] = ((r < q) & same)
    c[:, 4] = ((r <= q) & same)
    c[:, 5] = -1.0 * ((r > q) & same)
    c[:, 6] = same
    c[:, 7] = (r <= q)
    c[:, 8] = np.where(q >= r, 0.0, -30000.0)
    c[:, 9] = 1.0
    c[:, 10] = (q % 64 != 0)
    return c


def prep_shared(inp):
    f = lambda a: np.ascontiguousarray(np.asarray(a, dtype=np.float32))
    w_in = f(inp["w_in"])[0]
    sh = {}
    sh["ada_w"] = f(inp["ada_w"])[0]
    sh["ada_b"] = f(inp["ada_b"])[0][None, :]
    sh["nw1"] = f(inp["norm1_w"])
    sh["nw2"] = f(inp["norm2_w"])
    sh["fnw"] = f(inp["final_norm_w"])[None, :]
    wfm = np.zeros((NFM, 128, KD, 128), np.float32)
    for i, segs in enumerate(fm_tiles()):
        for (c0, n, r0) in segs:
            blk = w_in[:, c0:c0 + n].reshape(KD, 128, n).transpose(1, 0, 2)
            wfm[i, :, :, r0:r0 + n] = blk
    sh["w_fm"] = wfm
    sh["w_z"] = np.ascontiguousarray(w_in[:, 3360:4384].reshape(KD, 128, 1024).transpose(1, 0, 2))
    mu = f(inp["rwkv_mu"])[0]
    mu_fm = np.zeros((128, 27), np.float32)
    for i in range(24):
        mu_fm[:, i] = mu[i * 128:(i + 1) * 128]
    mu_fm[:, 24] = mu[3072:3200]
    mu_fm[:, 25] = mu[3200:3328]
    mu_fm[:32, 26] = mu[3328:3360]
    sh["mu_fm"] = mu_fm
    rs = np.zeros((128, 8, 5), np.float32)
    for j, nm in enumerate(["rwkv_w0", "rwkv_a0", "rwkv_k_k", "rwkv_k_a"]):
        rs[:, :, j] = f(inp[nm])[0].reshape(8, 128).T
    rs[:, :, 4] = f(inp["rwkv_r_k"])[0].reshape(8, 128).T
    sh["rw_small"] = rs
    sh["lora_w"] = np.concatenate([f(inp["rwkv_w_up"])[0], f(inp["rwkv_a_up"])[0]], axis=0)
    gu = f(inp["rwkv_g_up"])[0]
    sh["g_up0"] = np.ascontiguousarray(gu[:128])
    sh["g_up1"] = np.ascontiguousarray(gu[128:160])
    sh["ln_wb"] = np.stack([f(inp["rwkv_ln_w"])[0], f(inp["rwkv_ln_b"])[0]], axis=0)
    sh["consts"] = host_consts()
    cw = f(inp["mamba_conv_w"])[0]
    cb = f(inp["mamba_conv_b"])[0]
    cp = np.zeros((128, 12, 5), np.float32)
    for j in range(4):
        cp[:, :, j] = cw[j].reshape(12, 128).T
    cp[:, :, 4] = cb.reshape(12, 128).T
    sh["conv_p"] = cp
    sh["dt_p"] = np.stack([f(inp["mamba_dt_bias"])[0], f(inp["mamba_a_log"])[0]], axis=1)
    selh = np.zeros((16, 16, 128), np.float32)
    for h in range(16):
        selh[h, h, :] = 1.0
    sh["selh"] = selh
    sh["dsk_nw"] = np.stack([np.repeat(f(inp["mamba_d"])[0], 64), f(inp["mamba_norm_w"])[0]], axis=0)
    sh["w_o"] = np.ascontiguousarray(f(inp["w_out"])[0].reshape(KD, 128, D).transpose(1, 0, 2))
    wq = f(inp["peer_w_query"])[0]
    sh["wq_fm"] = np.ascontiguousarray(wq.reshape(KD, 128, 16, 128).transpose(2, 1, 0, 3))
    sk = f(inp["peer_sub_keys"])[0].reshape(16, 128, 128)
    sh["skT"] = np.ascontiguousarray(sk.transpose(2, 0, 1))
    zc = np.zeros((1, 256), np.float32)
    zc[0, 127] = 1.0
    sh["zc_in"] = zc
    sh["peer_u"] = f(inp["peer_u"])[0]
    sh["peer_v"] = f(inp["peer_v"])[0]
    return sh


def prep_core(inp, b, g, T, TB0):
    x = np.asarray(inp["x"], dtype=np.float32)[b]
    c = np.asarray(inp["c"], dtype=np.float32)[b]
    L = x.shape[0]
    m = {}
    if g == 1 or TB0 == 0:
        xs = x[:T]
        mask = np.ones((T,), np.float32)
    else:
        xs = np.concatenate([np.zeros((TB0, D), np.float32), x[:T - TB0]], axis=0)
        mask = np.concatenate([np.zeros((TB0,), np.float32), np.ones((T - TB0,), np.float32)])
    m["x_seq"] = np.ascontiguousarray(xs)
    m["maskp"] = np.ascontiguousarray(mask.reshape(T // 128, 128).T)
    m["mask16"] = np.ascontiguousarray(np.broadcast_to(mask[None, :], (16, T)))
    m["c_b"] = np.ascontiguousarray(c.reshape(KD, 128).T)
    return m


_NC_CACHE = {}


def kernel(**inputs):
    B, L, _ = inputs["x"].shape
    T, TB0 = L, L // 2
    sh = prep_shared(inputs)
    key = (T, TB0)
    if key not in _NC_CACHE:
        _NC_CACHE[key] = build_nc(T, TB0)
    nc = _NC_CACHE[key]
    in_maps = []
    for core in range(8):
        b, g = core // 2, core % 2
        m = dict(sh)
        m.update(prep_core(inputs, b, g, T, TB0))
        in_maps.append(m)
    res = run_bass_kernel_spmd(nc, in_maps, core_ids=list(range(8)))
    outp = np.zeros((B, L, D), np.float32)
    for core in range(8):
        b, g = core // 2, core % 2
        o = np.asarray(res.results[core]["out"], dtype=np.float32)
        outp[b, g * TB0:(g + 1) * TB0] = o
    return outp


GN_EPS = 64e-5


def stage_a2(st, tn):
    nc, p, T, NT, PS, psk, cst, cstb, A = (st[k] for k in ('nc', 'p', 'T', 'NT', 'PS', 'psk', 'cst', 'cstb', 'A'))
    h = H(p)
    IDF, NSU, NSL = cst[:, 0, :], cst[:, 1, :], cst[:, 5, :]
    MASK3 = cst[:, 2:5, :]
    IDB, BD1B, ONESB = cstb[:, 0, :], cstb[:, 6, :], cstb[:, 9, :]
    projT, oT = tn['projT'], tn['oT']
    al = A.alloc
    rw = al([128, 8, 5], F32)
    muT = al([128, 27], F32)
    lw = al([128, 1024], BF16)
    gu0 = al([128, 1024], BF16)
    gu1 = al([32, 1024], BF16)
    lnw = al([128, 1024], F32)
    lnb = al([128, 1024], F32)
    rst = al([128, 4, 128], F32)
    pl = al([128, 3, 513], F32)
    dlp = al([128, 3, 512], F32)
    lpl = al([128, 3, 512], F32)
    twb = al([128, 512], BF16)
    sg0 = al([128, 512], BF16)
    sg1 = al([32, 512], BF16)
    raw = al([128, 3, 513], F32)
    dl3 = al([128, 3, 512], F32)
    lp = al([128, 3, 512], F32)
    sgw, cs, cse, csm, Ep, En, Eex, Eend, aa, kk, kap, k2, bb, tmp, gT = [al([128, 512], F32) for _ in range(15)]
    kk2b = al([128, 512], BF16)
    KR = al([128, 8, 256], BF16)
    KKt, BT, KH, BHn, VT, PRD = [al([128, 8, 128], BF16) for _ in range(6)]
    TOK = al([128, 8, 384], BF16)
    M3 = al([128, 8, 384], BF16)
    QP = [al([128, 4, 256], F32) for _ in range(2)]
    W = [al([128, 4, 128], F32) for _ in range(2)]
    Wb = al([128, 8, 128], BF16)
    H32 = al([128, 8, 128], F32)
    Hb = al([128, 8, 128], BF16)
    Zb = al([128, 128], BF16)
    Ub = al([128, 128], BF16)
    O3 = al([128, 8, 128], F32)
    sq = al([128, 8, 128], F32)
    on = al([128, 8, 128], F32)
    stt_ = al([128, 8, 8], F32)
    ONb = al([128, 8, 128], BF16)
    oTr = al([128, 512], BF16)

    p.dma('sp', rw, tn['rw_small'][:, :, :], writes=['rw'])
    p.dma('sp', muT, tn['mu_fm'][:, :], writes=['muT'])
    p.dma('pool', lw, tn['lora_w'][:, :], writes=['lw'])
    p.dma('pool', gu0, tn['g_up0'][:, :], writes=['gu0'])
    p.dma('pool', gu1, tn['g_up1'][:, :], writes=['gu1'])
    p.dma('sp', lnw, tn['ln_wb'][0:1, :].to_broadcast([128, 1024]), writes=['lnw'])
    p.dma('sp', lnb, tn['ln_wb'][1:2, :].to_broadcast([128, 1024]), writes=['lnb'])
    h.cp('dve', rst, cst[:, 10, :].unsqueeze(1).to_broadcast([128, 4, 128]), ['cst'], ['rst'])
    rst2 = rst.rearrange("p a b -> p (a b)")
    for X, nm in ((KR, 'KR'), (KKt, 'KKt'), (BT, 'BT'), (KH, 'KH'), (BHn, 'BHn'), (VT, 'VT'), (PRD, 'PRD'), (H32, 'H32'), (Hb, 'Hb')):
        p.op('pool', (lambda X: (lambda e: e.memset(X, 0.0)))(X), writes=[nm])

    def v3(ap2, lo, hi):
        return ap2[lo:hi, :].rearrange("p (c t) -> p c t", c=8)

    HALF = ((0, 64), (64, 128))

    for it in range(NT):
        c0 = PAD + it * 512 - 1
        p.dma('sp', pl, projT[24:27, :, c0:c0 + 513].rearrange("n p c -> p n c"), reads=['projT'], writes=['pl'], sem='pl')
        h.tt('pool', dlp, pl[:, :, 0:512], pl[:, :, 1:513], ALU.subtract, ['pl'], ['dlp'])
        for n in range(3):
            h.stt(lpl[:, n, :], dlp[:, n, :], muT[:, 24 + n:25 + n], pl[:, n, 1:513], ALU.mult, ALU.add, ['dlp', 'pl', 'muT'], ['lpl'])
        h.act(twb[0:64, :], lpl[0:64, 0, :], AF.Tanh, ['lpl'], ['twb'])
        h.cp('act', twb[64:128, :], lpl[64:128, 0, :], ['lpl'], ['twb'])
        h.act(sg0, lpl[:, 1, :], AF.Sigmoid, ['lpl'], ['sg0'])
        h.act(sg1, lpl[0:32, 2, :], AF.Sigmoid, ['lpl'], ['sg1'])
        for hp in range(8):
            hc = slice(hp * 128, (hp + 1) * 128)
            for n in range(3):
                p.dma('sp' if n != 1 else 'act', raw[:, n, :], projT[n * 8 + hp, :, c0:c0 + 513], reads=['projT'], writes=['raw%d' % n], sem='raw%d' % n)
            h.tt('pool', dl3, raw[:, :, 0:512], raw[:, :, 1:513], ALU.subtract, ['raw0', 'raw1', 'raw2'], ['dl3'])
            for n in range(3):
                h.stt(lp[:, n, :], dl3[:, n, :], muT[:, n * 8 + hp:n * 8 + hp + 1], raw[:, n, 1:513], ALU.mult, ALU.add,
                      ['dl3', 'raw%d' % n, 'muT'], ['lp%d' % n])
            r_, k_, v_ = lp[:, 0, :], lp[:, 1, :], lp[:, 2, :]
            h.mm(PS[3][:, :], lw[0:64, hc], twb[0:64, :], ['lw', 'twb'], [psk(3)])
            h.act(sgw, PS[3][:, :], AF.Sigmoid, [psk(3), 'rw'], ['sgw'], bias=rw[:, hp, 0:1])
            h.mm(PS[7][:, :], lw[64:128, hc], twb[64:128, :], ['lw', 'twb'], [psk(7)])
            h.act(aa, PS[7][:, :], AF.Sigmoid, [psk(7), 'rw'], ['aa'], bias=rw[:, hp, 1:2])
            h.mm(PS[6][:, :], gu0[:, hc], sg0, ['gu0', 'sg0'], [psk(6)], start=True, stop=False)
            h.mm(PS[6][:, :], gu1[0:32, hc], sg1[0:32, :], ['gu1', 'sg1'], [psk(6)], start=False, stop=True)
            h.cp('dve', gT, PS[6][:, :], [psk(6)], ['gT'])
            p.op('dve', lambda e: e.tensor_tensor_scan(out=cs, data0=rst2, data1=sgw, initial=0.0, op0=ALU.mult, op1=ALU.add),
                 reads=['rst', 'sgw'], writes=['cs'])
            h.tt('pool', cse, cs, sgw, ALU.subtract, ['cs', 'sgw'], ['cse'])
            cs3 = cs.rearrange("p (c t) -> p c t", c=8)
            h.tt('dve', csm.rearrange("p (c t) -> p c t", c=8), cs3[:, :, 63:64].to_broadcast([128, 8, 64]), cs3, ALU.subtract, ['cs'], ['csm'])
            h.act(Ep, cs, AF.Exp, ['cs'], ['Ep'], scale=-C0)
            h.act(En, cs, AF.Exp, ['cs'], ['En'], scale=C0)
            h.act(Eex, cse, AF.Exp, ['cse'], ['Eex'], scale=-C0)
            h.act(Eend, csm, AF.Exp, ['csm'], ['Eend'], scale=-C0)
            h.ts('dve', kk, k_, rw[:, hp, 2:3], ALU.mult, ['lp1', 'rw'], ['kk'])
            h.act(kk2b, kk, AF.Square, ['kk'], ['kk2b'])
            h.mm(PS[3][:, :], BD1B, kk2b, ['cstb', 'kk2b'], [psk(3)])
            h.act(tmp, PS[3][:, :], AF.Sqrt, [psk(3)], ['tmp'])
            h.ts('dve', tmp, tmp, 1e-12, ALU.max, ['tmp'], ['tmp'])
            h.recip(tmp, tmp, ['tmp'], ['tmp'])
            h.tt('dve', kap, kk, tmp, ALU.mult, ['kk', 'tmp'], ['kap'])
            h.ts('dve', tmp, aa, 1.0, ALU.subtract, ['aa', 'rw', 'kap'], ['tmp'], s2=rw[:, hp, 3:4], op1=ALU.mult)
            h.stt(k2, tmp, 1.0, k_, ALU.add, ALU.mult, ['tmp', 'lp1'], ['k2'])
            h.tt('pool', bb, kap, aa, ALU.mult, ['kap', 'aa'], ['bb'])
            h.tt('pool', tmp, r_, k2, ALU.mult, ['lp0', 'k2'], ['tmp'])
            for lo, hi in HALF:
                h.ts('dve', PRD[lo:hi, :, lo:hi], v3(tmp, lo, hi), rw[lo:hi, hp, 4:5], ALU.mult, ['tmp', 'rw'], ['PRD'])
                h.tt('dve', KR[lo:hi, :, lo:hi], v3(kap, lo, hi), v3(Eex, lo, hi), ALU.mult, ['kap', 'Eex'], ['KR'])
                h.tt('pool', KR[lo:hi, :, 128 + lo:128 + hi], v3(r_, lo, hi), v3(Ep, lo, hi), ALU.mult, ['lp0', 'Ep'], ['KR'])
                h.tt('dve', KKt[lo:hi, :, lo:hi], v3(k2, lo, hi), v3(En, lo, hi), ALU.mult, ['k2', 'En'], ['KKt'])
                h.tt('pool', BT[lo:hi, :, lo:hi], v3(bb, lo, hi), v3(En, lo, hi), ALU.mult, ['bb', 'En'], ['BT'])
                h.tt('pool', KH[lo:hi, :, lo:hi], v3(k2, lo, hi), v3(Eend, lo, hi), ALU.mult, ['k2', 'Eend'], ['KH'])
                h.stt(BHn[lo:hi, :, lo:hi], v3(bb, lo, hi), -1.0, v3(Eend, lo, hi), ALU.mult, ALU.mult, ['bb', 'Eend'], ['BHn'])
                h.cp('act', VT[lo:hi, :, lo:hi], v3(v_, lo, hi), ['lp2'], ['VT'])
            PSb3 = PS[3].bitcast(BF16)
            for bt in range(2):
                QPa, Wa = QP[0], W[0]
                for ci in range(4):
                    c = bt * 4 + ci
                    bS = 4 + (c % 2)
                    h.mm(PS[bS][:, 0:256], BT[:, c, :], KR[:, c, :], ['BT', 'KR'], [psk(bS)])
                    h.mm(PS[bS][:, 256:512], KKt[:, c, :], KR[:, c, :], ['KKt', 'KR'], [psk(bS)])
                    h.mm(PS[7][:, ci * 128:(ci + 1) * 128], KR[:, c, 0:128], BT[:, c, :], ['KR', 'BT'], [psk(7)])
                    h.tt('dve', QPa[:, ci, 128:256], PS[bS][:, 0:128], NSU, ALU.mult, [psk(bS), 'cst'], ['QP0'])
                    h.tt('dve', M3[:, c, :], PS[bS][:, 128:512], MASK3.rearrange("p a b -> p (a b)"), ALU.mult, [psk(bS), 'cst'], ['M3'])
                h.tt('dve', QPa[:, :, 0:128], PS[7][:, :].rearrange("p (a b) -> p a b", a=4),
                     NSL.unsqueeze(1).to_broadcast([128, 4, 128]), ALU.mult, [psk(7), 'cst'], ['QP0'])
                h.tt('pool', Wa, QPa[:, :, 128:256], IDF.unsqueeze(1).to_broadcast([128, 4, 128]), ALU.add, ['QP0', 'cst'], ['W0'])
                for lv in range(5):
                    cur, nxt = QP[lv % 2], QP[(lv + 1) % 2]
                    ck, nk = 'QP%d' % (lv % 2), 'QP%d' % ((lv + 1) % 2)
                    Wc, Wn = W[lv % 2], W[(lv + 1) % 2]
                    wck, wnk = 'W%d' % (lv % 2), 'W%d' % ((lv + 1) % 2)
                    for ci in range(4):
                        bk = ci // 2
                        o0 = (ci % 2) * 256
                        h.mm(PS[bk][:, o0:o0 + 128], cur[:, ci, 128:256], cur[:, ci, 0:128], [ck], [psk(bk)])
                        if lv < 4:
                            h.mm(PS[bk][:, o0 + 128:o0 + 256], cur[:, ci, 0:128], cur[:, ci, 128:256], [ck], [psk(bk)])
                    w_ = 256 if lv < 4 else 128
                    for bk, eng in ((0, 'act'), (1, 'dve')):
                        h.cp(eng, nxt[:, 2 * bk:2 * bk + 2, 0:w_], PS[bk][:, :].rearrange("p (a b) -> p a b", a=2)[:, :, 0:w_], [psk(bk)], [nk])
                    for ci in range(4):
                        h.mm(PS[2][:, ci * 128:(ci + 1) * 128], nxt[:, ci, 0:128], Wc[:, ci, :], [nk, wck], [psk(2)])
                    if lv < 4:
                        h.tt('dve', Wn, PS[2][:, :].rearrange("p (a b) -> p a b", a=4), Wc, ALU.add, [psk(2), wck], [wnk])
                    else:
                        h.tt('dve', Wb[:, bt * 4:bt * 4 + 4, :], PS[2][:, :].rearrange("p (a b) -> p a b", a=4), Wc, ALU.add, [psk(2), wck], ['Wb'])
                for pr in range(2):
                    for cc in range(2):
                        c = bt * 4 + pr * 2 + cc
                        for kq, (X, nm) in enumerate(((VT, 'VT'), (KH, 'KH'), (BHn, 'BHn'))):
                            o0 = cc * 384 + kq * 128
                            h.tr(PSb3[:, o0:o0 + 128], X[:, c, :], IDB, [nm, 'cstb'], [psk(3)])
                    c_lo = bt * 4 + pr * 2
                    h.cp('act', TOK[:, c_lo:c_lo + 2, :], PSb3[:, 0:768].rearrange("p (a b) -> p a b", a=2), [psk(3)], ['TOK'])
            Hbh, H32h = Hb[:, hp, :], H32[:, hp, :]
            for c in range(8):
                Vt = TOK[:, c, 0:128]
                h.mm(PS[0][:, 0:128], KR[:, c, 0:128], Hbh, ['KR', 'Hb'], [psk(0)], start=True, stop=False)
                h.mm(PS[0][:, 0:128], M3[:, c, 128:256], Vt, ['M3', 'TOK'], [psk(0)], start=False, stop=True)
                h.cp('act', Zb, PS[0][:, 0:128], [psk(0)], ['Zb'])
                h.mm(PS[0][:, 128:256], Wb[:, c, :], Zb, ['Wb', 'Zb'], [psk(0)])
                h.cp('dve', Ub, PS[0][:, 128:256], [psk(0)], ['Ub'])
                oc = slice((c % 4) * 128, (c % 4 + 1) * 128)
                h.mm(PS[1][:, oc], KR[:, c, 128:256], Hbh, ['KR', 'Hb'], [psk(1)], start=True, stop=False)
                h.mm(PS[1][:, oc], M3[:, c, 256:384], Vt, ['M3', 'TOK'], [psk(1)], start=False, stop=False)
                h.mm(PS[1][:, oc], M3[:, c, 0:128], Ub, ['M3', 'Ub'], [psk(1)], start=False, stop=True)
                if c % 4 == 3:
                    h.cp('act', O3[:, c - 3:c + 1, :], PS[1][:, :].rearrange("p (a b) -> p a b", a=4), [psk(1)], ['O3'])
                h.mm(PS[2][:, 0:128], TOK[:, c, 128:256], Vt, ['TOK'], [psk(2)], start=True, stop=False)
                h.mm(PS[2][:, 0:128], TOK[:, c, 256:384], Ub, ['TOK', 'Ub'], [psk(2)], start=False, stop=True)
                h.stt(H32h, H32h, Ep[:, c * 64 + 63:c * 64 + 64], PS[2][:, 0:128], ALU.mult, ALU.add, ['H32', 'Ep', psk(2)], ['H32'])
                h.cp('act', Hbh, H32h, ['H32'], ['Hb'])
            s1, s2, mean, msq, var, rstd, bs = (stt_[:, :, i] for i in range(7))
            h.red(s1, O3, ['O3'], ['st1'])
            h.ts('dve', mean, s1, 1.0 / 64, ALU.mult, ['st1'], ['mean'])
            h.tt('dve', on, O3, mean.unsqueeze(2).to_broadcast([128, 8, 128]), ALU.subtract, ['O3', 'mean'], ['on'])
            h.tt('pool', sq, on, on, ALU.mult, ['on'], ['sq'])
            for lo, hi in HALF:
                h.red(s2[lo:hi, :], sq[lo:hi, :, lo:hi], ['sq'], ['st2'])
            h.ts('dve', var, s2, 1.0 / 64, ALU.mult, ['st2'], ['var'], s2=GN_EPS, op1=ALU.add)
            h.act(var, var, AF.Sqrt, ['var'], ['var'])
            h.recip(rstd, var, ['var'], ['rstd'])
            h.tt('pool', on, on, rstd.unsqueeze(2).to_broadcast([128, 8, 128]), ALU.mult, ['on', 'rstd'], ['on'])
            h.tt('dve', on, on, lnw[:, hc].unsqueeze(1).to_broadcast([128, 8, 128]), ALU.mult, ['on', 'lnw'], ['on'])
            h.tt('pool', on, on, lnb[:, hc].unsqueeze(1).to_broadcast([128, 8, 128]), ALU.add, ['on', 'lnb'], ['on'])
            for c in range(8):
                h.mm(PS[7][:, c:c + 1], PRD[:, c, :], ONESB[:, 0:1], ['PRD', 'cstb'], [psk(7)])
            h.cp('dve', bs, PS[7][:, 0:8], [psk(7)], ['bs'])
            h.tt('dve', sq, TOK[:, :, 0:128], bs.unsqueeze(2).to_broadcast([128, 8, 128]), ALU.mult, ['TOK', 'bs'], ['sq'])
            h.tt('pool', ONb, on, sq, ALU.add, ['on', 'sq'], ['ONb'])
            for c in range(8):
                h.tr(PSb3[:, c * 128:(c + 1) * 128], ONb[:, c, :], IDB, ['ONb', 'cstb'], [psk(3)])
            P3 = PSb3[:, :].rearrange("p (c t) -> p c t", c=8)
            for lo, hi in HALF:
                h.tt('dve', v3(oTr, lo, hi), P3[lo:hi, :, lo:hi], v3(gT, lo, hi), ALU.mult, [psk(3), 'gT'], ['oTr'])
            p.dma('sp', oT[hp * 128:(hp + 1) * 128, it * 512:(it + 1) * 512], oTr, reads=['oTr'], writes=['oT'], sem='oTr')


def stage_a3(st, tn):
    nc, p, T, NT, PS, psk, cst, cstb, A = (st[k] for k in ('nc', 'p', 'T', 'NT', 'PS', 'psk', 'cst', 'cstb', 'A'))
    h = H(p)
    IDF, TRI, MNEG, ONESF = cst[:, 0, :], cst[:, 7, :], cst[:, 8, :], cst[:, 9, :]
    IDB = cstb[:, 0, :]
    projT, oT, zscr = tn['projT'], tn['oT'], tn['zscr']
    al = A.alloc
    cvp = al([128, 12, 5], F32)
    dtp = al([48, 2], F32)
    An = al([48, 1], F32)
    selt = al([128, 16, 128], F32)
    dsk = al([128, 1024], F32)
    nwb = al([128, 1024], F32)
    m48 = al([48, 512], F32)
    xr = [al([128, 515], F32) for _ in range(2)]
    acc = [al([128, 512], F32) for _ in range(2)]
    xsb = al([128, 8, 512], BF16)
    BTc = al([128, 2, 512], BF16)
    CTc = al([128, 2, 512], BF16)
    dtt = al([48, 512], F32)
    dta = al([128, 48], F32)
    acs, nacs, eacs, dch, wst, t16 = [al([128, 16], F32) for _ in range(6)]
    acsT = al([128, 128], F32)
    xtok = al([128, 1024], BF16)
    xdt = al([128, 1024], BF16)
    xdtw = al([128, 1024], BF16)
    ztok = al([128, 1024], F32)
    y = al([128, 1024], F32)
    ytmp = al([128, 1024], F32)
    yb = al([128, 1024], BF16)
    CBt = al([128, 2, 128], F32)
    Lt = [al([128, 128], F32) for _ in range(2)]
    Mh = [al([128, 128], BF16) for _ in range(2)]
    Btok = al([128, 2, 128], BF16)
    ST32 = al([128, 2, 512], F32)
    STb = al([128, 2, 512], BF16)
    oTs = al([128, 8, 512], BF16)
    ssn = al([128, 4], F32)

    p.dma('sp', cvp, tn['conv_p'][:, :, :], writes=['cvp'])
    p.op('pool', lambda e: e.memset(dtp, 0.0), writes=['dtp'])
    p.op('pool', lambda e: e.memset(dtt, 0.0), writes=['dtt'])
    p.op('pool', lambda e: e.memset(m48, 0.0), writes=['m48'])
    p.op('pool', lambda e: e.memset(ST32, 0.0), writes=['ST32'])
    p.op('pool', lambda e: e.memset(STb, 0.0), writes=['STb'])
    p.dma('sp', dtp[0:16, :], tn['dt_p'][:, :], reads=[], writes=['dtp'])
    p.dma('sp', dtp[32:48, :], tn['dt_p'][:, :], reads=[], writes=['dtp'])
    p.op('pool', lambda e: e.memset(selt, 0.0), writes=['selt'])
    p.op('pool', lambda e: e.memset(acsT, 0.0), writes=['acsT'])
    p.dma('sp', selt[0:16, :, :], tn['selh'][:, :, :], writes=['selt'])
    p.dma('sp', dsk, tn['dsk_nw'][0:1, :].to_broadcast([128, 1024]), writes=['dsk'])
    p.dma('sp', nwb, tn['dsk_nw'][1:2, :].to_broadcast([128, 1024]), writes=['nwb'])
    h.act(An, dtp[:, 1:2], AF.Exp, ['dtp'], ['An'])
    h.ts('dve', An, An, -1.0, ALU.mult, ['An'], ['An'])
    PSb1 = PS[1].bitcast(BF16)
    PSb2 = PS[2].bitcast(BF16)
    nx = 0
    if A3_LEVEL < 2:
        return
    for it in range(NT):
        c0 = PAD + it * 512 - 3
        for i in range(12):
            b = nx % 2
            nx += 1
            p.dma('sp' if i % 2 == 0 else 'act', xr[b], projT[27 + i, :, c0:c0 + 515], reads=['projT'], writes=['xr%d' % b], sem='xr%d' % b)
            ac = acc[b]
            h.ts('dve', ac, xr[b][:, 0:512], cvp[:, i, 0:1], ALU.mult, ['xr%d' % b, 'cvp'], ['acc%d' % b], s2=cvp[:, i, 4:5], op1=ALU.add)
            for j in (1, 2, 3):
                h.stt(ac, xr[b][:, j:j + 512], cvp[:, i, j:j + 1], ac, ALU.mult, ALU.add, ['xr%d' % b, 'cvp', 'acc%d' % b], ['acc%d' % b])
            if i < 8:
                dst, dk = xsb[:, i, :], 'xsb'
            elif i < 10:
                dst, dk = BTc[:, i - 8, :], 'BTc'
            else:
                dst, dk = CTc[:, i - 10, :], 'CTc'
            h.act(dst, ac, AF.Silu, ['acc%d' % b], [dk])
        tc0 = PAD + it * 512
        p.dma('sp', dtt[0:16, :], projT[39, 0:16, tc0:tc0 + 512], reads=['projT'], writes=['dtt'], sem='dtt')
        p.dma('sp', dtt[32:48, :], projT[39, 0:16, tc0:tc0 + 512], reads=['projT'], writes=['dtt'], sem='dtt2')
        p.dma('act', m48[0:16, :], tn['mask16'][:, it * 512:(it + 1) * 512], writes=['m48'], sem='m48')
        p.dma('act', m48[32:48, :], tn['mask16'][:, it * 512:(it + 1) * 512], writes=['m48'], sem='m48b')
        h.act(dtt, dtt, AF.Exp, ['dtt', 'dtp'], ['dtt'], bias=dtp[:, 0:1])
        h.act(dtt, dtt, AF.Ln, ['dtt'], ['dtt'], bias=1.0)
        h.tt('dve', dtt, dtt, m48, ALU.mult, ['dtt', 'm48'], ['dtt'])
        h.ts('dve', dtt[32:48, :], dtt[32:48, :], An[32:48, 0:1], ALU.mult, ['dtt', 'An'], ['dtt'])
        if A3_LEVEL < 3:
            continue
        for ck in range(4):
            cc = slice(ck * 128, (ck + 1) * 128)
            t0 = it * 512 + ck * 128
            p.dma('act', ztok, zscr[t0:t0 + 128, :], reads=['zscr'], writes=['ztok'], sem='ztok')
            h.tr(PS[0][:, 0:48], dtt[0:48, cc], IDF[0:48, 0:48], ['dtt', 'cst'], [psk(0)])
            h.cp('dve', dta, PS[0][:, 0:48], [psk(0)], ['dta'])
            if A3_LEVEL < 3.01:
                continue
            h.mm(PS[0][:, 64:80], TRI, dta[:, 32:48], ['cst', 'dta'], [psk(0)])
            h.mm(PS[0][0:16, 128:256], dta[:, 32:48], TRI, ['cst', 'dta'], [psk(0)])
            h.mm(PS[0][:, 96:112], ONESF, dta[:, 32:48], ['cst', 'dta'], [psk(0)])
            h.cp('dve', acs, PS[0][:, 64:80], [psk(0)], ['acs'])
            h.ts('dve', nacs, acs, -1.0, ALU.mult, ['acs'], ['nacs'])
            if A3_LEVEL < 3.02:
                continue
            h.act(eacs, acs, AF.Exp, ['acs'], ['eacs'])
            h.act(dch, PS[0][:, 96:112], AF.Exp, [psk(0)], ['dch'])
            if A3_LEVEL < 3.03:
                continue
            h.cp('act', t16, PS[0][:, 96:112], [psk(0)], ['t16'])
            h.tt('dve', t16, t16, acs, ALU.subtract, ['t16', 'acs'], ['t16'])
            if A3_LEVEL < 3.032:
                continue
            h.act(wst, t16, AF.Exp, ['t16'], ['wst'])
            if A3_LEVEL < 3.04:
                continue
            h.cp('act', acsT[0:16, :], PS[0][0:16, 128:256], [psk(0)], ['acsT'])
            if A3_LEVEL < 3.1:
                continue
            for i in range(8):
                h.tr(PSb1[:, i * 128:(i + 1) * 128], xsb[:, i, cc], IDB, ['xsb', 'cstb'], [psk(1)])
            h.cp('act', xtok, PSb1[:, :], [psk(1)], ['xtok'])
            h.tt('dve', xdt.rearrange("p (h q) -> p h q", h=16), xtok.rearrange("p (h q) -> p h q", h=16),
                 dta[:, 0:16].unsqueeze(2).to_broadcast([128, 16, 64]), ALU.mult, ['xtok', 'dta'], ['xdt'])
            h.tt('pool', xdtw.rearrange("p (h q) -> p h q", h=16), xdt.rearrange("p (h q) -> p h q", h=16),
                 wst.unsqueeze(2).to_broadcast([128, 16, 64]), ALU.mult, ['xdt', 'wst'], ['xdtw'])
            if A3_LEVEL < 3.2:
                continue
            for g in range(2):
                h.tr(PSb2[:, g * 128:(g + 1) * 128], BTc[:, g, cc], IDB, ['BTc', 'cstb'], [psk(2)])
            h.cp('act', Btok, PSb2[:, 0:256].rearrange("p (g n) -> p g n", g=2), [psk(2)], ['Btok'])
            for g in range(2):
                h.mm(PS[3][:, g * 128:(g + 1) * 128], BTc[:, g, cc], CTc[:, g, cc], ['BTc', 'CTc'], [psk(3)])
            h.cp('dve', CBt, PS[3][:, 0:256].rearrange("p (g n) -> p g n", g=2), [psk(3)], ['CBt'])
            if A3_LEVEL < 4:
                continue
            for g in range(2):
                for r in range(8):
                    hh = g * 8 + r
                    lb = hh % 2
                    bL = 4 + lb
                    h.mm(PS[bL][:, 0:128], selt[:, hh, :], acsT, ['selt', 'acsT'], [psk(bL)], start=True, stop=False)
                    h.mm(PS[bL][:, 0:128], IDF, MNEG, ['cst'], [psk(bL)], start=False, stop=True)
                    h.act(Lt[lb], PS[bL][:, 0:128], AF.Exp, [psk(bL), 'nacs'], ['Lt%d' % lb], bias=nacs[:, hh:hh + 1])
                    h.tt('dve' if lb == 0 else 'pool', Mh[lb], CBt[:, g, :], Lt[lb], ALU.mult, ['CBt', 'Lt%d' % lb], ['Mh%d' % lb])
                    h.mm(PS[6][:, r * 64:(r + 1) * 64], Mh[lb], xdt[:, hh * 64:(hh + 1) * 64], ['Mh%d' % lb, 'xdt'], [psk(6)])
                h.mm(PS[7][:, :], CTc[:, g, cc], STb[:, g, :], ['CTc', 'STb'], [psk(7)])
                gs = slice(g * 512, (g + 1) * 512)
                h.cp('act', ytmp[:, gs], PS[7][:, :], [psk(7)], ['ytmp'])
                h.tt('dve', ytmp[:, gs].rearrange("p (h q) -> p h q", h=8), ytmp[:, gs].rearrange("p (h q) -> p h q", h=8),
                     eacs[:, g * 8:(g + 1) * 8].unsqueeze(2).to_broadcast([128, 8, 64]), ALU.mult, ['ytmp', 'eacs'], ['ytmp'])
                h.tt('dve', y[:, gs], PS[6][:, :], ytmp[:, gs], ALU.add, ['ytmp', psk(6)], ['y'])
            if A3_LEVEL < 5:
                continue
            h.tt('pool', ytmp, xtok, dsk, ALU.mult, ['xtok', 'dsk'], ['ytmp'])
            h.tt('pool', y, y, ytmp, ALU.add, ['y', 'ytmp'], ['y'])
            h.act(ztok, ztok, AF.Silu, ['ztok'], ['ztok'])
            h.tt('dve', y, y, ztok, ALU.mult, ['y', 'ztok'], ['y'])
            if A3_LEVEL < 5.1:
                continue
            for g in range(2):
                gs = slice(g * 512, (g + 1) * 512)
                h.act(ytmp[:, gs], y[:, gs], AF.Square, ['y'], ['ytmp', 'ssn'], accum_out=ssn[:, g:g + 1])
            h.act(ssn[:, 2:4], ssn[:, 0:2], AF.Sqrt, ['ssn'], ['ssn'], scale=1.0 / 512, bias=1e-5)
            h.recip(ssn[:, 2:4], ssn[:, 2:4], ['ssn'], ['ssn'])
            h.tt('dve', y.rearrange("p (g q) -> p g q", g=2), y.rearrange("p (g q) -> p g q", g=2),
                 ssn[:, 2:4].unsqueeze(2).to_broadcast([128, 2, 512]), ALU.mult, ['y', 'ssn'], ['y'])
            h.tt('pool', yb, y, nwb, ALU.mult, ['y', 'nwb'], ['yb'])
            if A3_LEVEL < 5.2:
                continue
            for i in range(8):
                h.tr(PSb2[:, i * 128:(i + 1) * 128], yb[:, i * 128:(i + 1) * 128], IDB, ['yb', 'cstb'], [psk(2)])
            h.cp('act', oTs[:, :, cc], PSb2[:, :].rearrange("p (i t) -> p i t", i=8), [psk(2)], ['oTs'])
            if A3_LEVEL < 5.3:
                continue
            for g in range(2):
                h.mm(PS[3][:, :], Btok[:, g, :], xdtw[:, g * 512:(g + 1) * 512], ['Btok', 'xdtw'], [psk(3)])
                S3v = ST32[:, g, :].rearrange("p (h q) -> p h q", h=8)
                h.tt('pool', S3v, S3v, dch[:, g * 8:(g + 1) * 8].unsqueeze(2).to_broadcast([128, 8, 64]), ALU.mult, ['ST32', 'dch'], ['ST32'])
                h.cp('act', ytmp[:, 0:512], PS[3][:, :], [psk(3)], ['ytmp'])
                h.tt('dve', ST32[:, g, :], ST32[:, g, :], ytmp[:, 0:512], ALU.add, ['ST32', 'ytmp'], ['ST32'])
                h.cp('act', STb[:, g, :], ST32[:, g, :], ['ST32'], ['STb'])
        p.dma('sp', oT[1024:2048, it * 512:(it + 1) * 512].rearrange("(i p) t -> p i t", p=128), oTs, reads=['oTs'], writes=['oT'], sem='oTs')


def stage_b(st, tn):
    nc, p, T, TB0, PS, psk, cst, cstb, A, psall = (st[k] for k in ('nc', 'p', 'T', 'TB0', 'PS', 'psk', 'cst', 'cstb', 'A', 'psall'))
    h = H(p)
    IDF = cst[:, 0, :]
    IDB = cstb[:, 0, :]
    x_seq, oT, out = tn['x_seq'], tn['oT'], tn['out']
    ubf, vbf = tn['ubf'], tn['vbf']
    al = A.alloc
    g1b, w2s, sh2b, g2b, fnwb = [al([128, D], F32) for _ in range(5)]
    oTt = al([128, KD, 128], BF16)
    wo_t = [al([128, KD, 512], BF16) for _ in range(2)]
    hb = al([128, D], F32)
    outb = al([128, D], F32)
    tmpb = [al([128, 512], F32) for _ in range(2)]
    n2b = al([128, D], BF16)
    n2T = al([128, KD, 128], BF16)
    wq = [al([128, KD, 128], BF16) for _ in range(2)]
    qT = al([128, 16, 128], BF16)
    skt = al([128, 16, 128], BF16)
    Sc = al([128, D], F32)
    work = al([128, D], F32)
    tv = al([128, 16, 16], F32)
    ti = al([128, 16, 16], U32)
    tif = al([128, 16, 16], F32)
    ti1s = al([128, 8, 16], F32)
    cs_ = al([128, 8, 256], F32)
    eqb = al([128, 16, 256], BF16)
    prb = al([128, 16, 256], BF16)
    ehl = al([128, 2, 128], F32)
    tb4 = al([128, 4, 128], BF16)
    trf = al([128, 4, 128], F32)
    gres = al([128, 128], F32)
    ts_ = al([128, 8, 16], F32)
    ee = al([128, 8, 16], F32)
    gates = al([128, 128], F32)
    eidx = al([128, 128], F32)
    zz = al([128, 16], F32)
    eT = al([128, 128], U32)
    gTt = al([128, 128], F32)
    Ug = [al([128, D], BF16) for _ in range(2)]
    Vg = [al([128, D], BF16) for _ in range(2)]
    junk = [al([128, 1024], BF16) for _ in range(2)]
    accs = al([128, 8], F32)
    Wl = [al([128, 128], BF16) for _ in range(2)]
    Zc = al([128, 256], BF16)
    cvb = Ug
    sm = al([128, 8], F32)

    for i_, (tile, key) in enumerate(((w2s, 3), (sh2b, 4), (g1b, 2), (g2b, 5))):
        p.dma('sp' if i_ % 2 else 'act', tile, tn['modrow'][key:key + 1, :].to_broadcast([128, D]), reads=['modrow'], writes=['bcB%d' % key])
    p.dma('sp', fnwb, tn['fnw'][0:1, :].to_broadcast([128, D]), writes=['fnwb'])
    p.dma('pool', skt, tn['skT'][:, :, :], writes=['skt'])
    p.dma('pool', Zc, tn['zc_in'][0:1, :].to_broadcast([128, 256]), writes=['Zc'])
    for tbl, dst, nm in ((tn['peer_u'], ubf, 'ubf'), (tn['peer_v'], vbf, 'vbf')):
        for n in range(NEXP // 128):
            b = n % 2
            p.dma('pool', cvb[b], tbl[n * 128:(n + 1) * 128, :], writes=['Ug%d' % b], sem='cvl%d' % b)
            p.dma('sp' if b == 0 else 'act', dst[n * 128:(n + 1) * 128, :], cvb[b], reads=['Ug%d' % b], writes=[nm], sem='cvs%d' % b)

    nwo = 0
    nwq = 0
    if B_LEVEL < 2:
        return
    for blk in range(TB0 // 128, T // 128):
        t0 = blk * 128
        ob = blk - TB0 // 128
        p.dma('sp', hb, x_seq[t0:t0 + 128, :], writes=['hb'], sem='hb')
        p.dma('act', oTt, oT[:, t0:t0 + 128].rearrange("(j q) t -> q j t", q=128), reads=['oT'], writes=['oTt'], sem='oTt')
        for ct in range(4):
            wb = nwo % 2
            nwo += 1
            cs4 = slice(ct * 512, (ct + 1) * 512)
            p.dma('pool', wo_t[wb], tn['w_o'][:, :, cs4], writes=['wo%d' % wb], sem='wo%d' % wb)
            bk = ct % 2
            for j in range(KD):
                h.mm(PS[bk], oTt[:, j, :], wo_t[wb][:, j, :], ['oTt', 'wo%d' % wb], [psk(bk)], start=(j == 0), stop=(j == KD - 1))
            h.cp('act', tmpb[bk], PS[bk], [psk(bk)], ['tmpb%d' % bk])
            h.tt('dve', tmpb[bk], tmpb[bk], g1b[:, cs4], ALU.mult, ['tmpb%d' % bk, 'bcB2'], ['tmpb%d' % bk])
            h.tt('pool', hb[:, cs4], hb[:, cs4], tmpb[bk], ALU.add, ['hb', 'tmpb%d' % bk], ['hb'])
        h.act(outb, hb, AF.Square, ['hb'], ['outb', 'sm'], accum_out=sm[:, 0:1])
        h.act(sm[:, 1:2], sm[:, 0:1], AF.Sqrt, ['sm'], ['sm'], scale=1.0 / D, bias=1e-6)
        h.recip(sm[:, 2:3], sm[:, 1:2], ['sm'], ['sm'])
        h.stt(outb, hb, sm[:, 2:3], w2s, ALU.mult, ALU.mult, ['hb', 'sm', 'bcB3'], ['outb'])
        h.tt('pool', n2b, outb, sh2b, ALU.add, ['outb', 'bcB4'], ['n2b'])
        for q in range(2):
            pb = PS[2 + q].bitcast(BF16)
            for jj in range(8):
                j = q * 8 + jj
                h.tr(pb[:, jj * 128:(jj + 1) * 128], n2b[:, j * 128:(j + 1) * 128], IDB, ['n2b', 'cstb'], [psk(2 + q)])
            h.cp('act' if q else 'dve', n2T[:, q * 8:(q + 1) * 8, :], pb[:, :].rearrange("q (j t) -> q j t", j=8), [psk(2 + q)], ['n2T'])
        if B_LEVEL < 3:
            continue
        for hp16 in range(16):
            wb = nwq % 2
            nwq += 1
            p.dma('pool', wq[wb], tn['wq_fm'][hp16, :, :, :], writes=['wq%d' % wb], sem='wq%d' % wb)
            bk = 4 + hp16 % 2
            for j in range(KD):
                h.mm(PS[bk][:, 0:128], wq[wb][:, j, :], n2T[:, j, :], ['wq%d' % wb, 'n2T'], [psk(bk)], start=(j == 0), stop=(j == KD - 1))
            h.cp('act' if hp16 % 2 else 'dve', qT[:, hp16, :], PS[bk][:, 0:128], [psk(bk)], ['qT'])
        for hp16 in range(16):
            bk = hp16 // 4
            h.mm(PS[bk][:, (hp16 % 4) * 128:(hp16 % 4 + 1) * 128], qT[:, hp16, :], skt[:, hp16, :], ['qT', 'skt'], [psk(bk)])
        for bk in range(4):
            h.cp('act' if bk % 2 else 'dve', Sc[:, bk * 512:(bk + 1) * 512], PS[bk], [psk(bk)], ['Sc'])
        if B_LEVEL < 4:
            continue
        for hp16 in range(16):
            sl = slice(hp16 * 128, (hp16 + 1) * 128)
            p.op('dve', (lambda hp16=hp16, sl=sl: (lambda e: e.max(out=tv[:, hp16, 0:8], in_=Sc[:, sl])))(), reads=['Sc'], writes=['tv'])
            p.op('dve', (lambda hp16=hp16, sl=sl: (lambda e: e.max_index(out=ti[:, hp16, 0:8], in_max=tv[:, hp16, 0:8], in_values=Sc[:, sl])))(), reads=['Sc', 'tv'], writes=['ti'])
            p.op('dve', (lambda hp16=hp16, sl=sl: (lambda e: e.match_replace(out=work[:, sl], in_to_replace=tv[:, hp16, 0:8], in_values=Sc[:, sl], imm_value=-1e30)))(), reads=['Sc', 'tv'], writes=['work'])
            p.op('dve', (lambda hp16=hp16, sl=sl: (lambda e: e.max(out=tv[:, hp16, 8:16], in_=work[:, sl])))(), reads=['work'], writes=['tv'])
            p.op('dve', (lambda hp16=hp16, sl=sl: (lambda e: e.max_index(out=ti[:, hp16, 8:16], in_max=tv[:, hp16, 8:16], in_values=work[:, sl])))(), reads=['work', 'tv'], writes=['ti'])
        h.cp('dve', tif, ti, ['ti'], ['tif'])
        if B_LEVEL < 4.1:
            continue
        tv4 = tv.rearrange("q (a b) k -> q a b k", b=2)
        tif4 = tif.rearrange("q (a b) k -> q a b k", b=2)
        cs4v = cs_.rearrange("q a (k m) -> q a k m", k=16)
        h.tt('dve', cs4v, tv4[:, :, 0, :].unsqueeze(3).to_broadcast([128, 8, 16, 16]), tv4[:, :, 1, :].unsqueeze(2).to_broadcast([128, 8, 16, 16]),
             ALU.add, ['tv'], ['cs_'])
        if B_LEVEL < 4.2:
            continue
        for hh in range(8):
            sl = slice(hh * 256, (hh + 1) * 256)
            p.op('dve', (lambda hh=hh: (lambda e: e.max(out=ts_[:, hh, 0:8], in_=cs_[:, hh, :])))(), reads=['cs_'], writes=['ts_'])
            p.op('dve', (lambda hh=hh, sl=sl: (lambda e: e.match_replace(out=work[:, sl], in_to_replace=ts_[:, hh, 0:8], in_values=cs_[:, hh, :], imm_value=-1e30)))(), reads=['cs_', 'ts_'], writes=['work'])
            p.op('dve', (lambda hh=hh, sl=sl: (lambda e: e.max(out=ts_[:, hh, 8:16], in_=work[:, sl])))(), reads=['work'], writes=['ts_'])
        if B_LEVEL < 4.3:
            continue
        for hh in range(8):
            h.tt('dve', eqb, cs_[:, hh, :].unsqueeze(1).to_broadcast([128, 16, 256]), ts_[:, hh, :].unsqueeze(2).to_broadcast([128, 16, 256]),
                 ALU.is_equal, ['cs_', 'ts_'], ['eqb'])
            eq4 = eqb.rearrange("q k (a b) -> q k a b", a=16)
            pr4 = prb.rearrange("q k (a b) -> q k a b", a=16)
            h.tt('pool', pr4, eq4, tif4[:, hh, 0, :].unsqueeze(1).unsqueeze(3).to_broadcast([128, 16, 16, 16]), ALU.mult, ['eqb', 'tif'], ['prb'])
            h.red(ehl[:, 0, hh * 16:(hh + 1) * 16], prb, ['prb'], ['ehl'], op=ALU.max)
            h.tt('pool', pr4, eq4, tif4[:, hh, 1, :].unsqueeze(1).unsqueeze(2).to_broadcast([128, 16, 16, 16]), ALU.mult, ['eqb', 'tif'], ['prb'])
            h.red(ehl[:, 1, hh * 16:(hh + 1) * 16], prb, ['prb'], ['ehl'], op=ALU.max)
        if B_LEVEL < 4.4:
            continue
        h.tt('dve', ee, ts_, ts_[:, :, 0:1].to_broadcast([128, 8, 16]), ALU.subtract, ['ts_'], ['ee'])
        h.act(ee, ee, AF.Exp, ['ee'], ['ee'])
        h.red(zz[:, 0:8], ee, ['ee'], ['zz'])
        h.recip(zz[:, 8:16], zz[:, 0:8], ['zz'], ['zz'])
        h.tt('dve', gates.rearrange("q (a k) -> q a k", a=8), ee, zz[:, 8:16].unsqueeze(2).to_broadcast([128, 8, 16]), ALU.mult, ['ee', 'zz'], ['gates'])
        if B_LEVEL < 4.45:
            continue
        h.cp('dve', tb4[:, 0:2, :], ehl, ['ehl'], ['tb4'])
        h.cp('act', tb4[:, 2, :], gates, ['gates'], ['tb4'])
        h.tt('dve', gres, gates, tb4[:, 2, :], ALU.subtract, ['gates', 'tb4'], ['gres'])
        h.cp('act', tb4[:, 3, :], gres, ['gres'], ['tb4'])
        pb4 = PS[4].bitcast(BF16)
        for k_ in range(4):
            h.tr(pb4[:, k_ * 128:(k_ + 1) * 128], tb4[:, k_, :], IDB, ['tb4', 'cstb'], [psk(4)])
        h.cp('act', trf, pb4[:, 0:512].rearrange("q (a b) -> q a b", a=4), [psk(4)], ['trf'])
        h.stt(eidx, trf[:, 0, :], 128.0, trf[:, 1, :], ALU.mult, ALU.add, ['trf'], ['eidx'])
        h.cp('dve', eT, eidx, ['eidx'], ['eT'])
        h.tt('dve', gTt, trf[:, 2, :], trf[:, 3, :], ALU.add, ['trf'], ['gTt'])
        if B_LEVEL < 5:
            continue
        for l in range(128):
            s_ = l % 2
            p.dma_fn('pool', (lambda s_=s_, l=l: (lambda e: e.indirect_dma_start(
                out=Ug[s_], out_offset=None, in_=ubf[:, :], in_offset=bass.IndirectOffsetOnAxis(ap=eT[:, l:l + 1], axis=0))))(),
                reads=['eT', 'ubf'], writes=['Ug%d' % s_], sem='ug%d' % s_)
            p.dma_fn('pool', (lambda s_=s_, l=l: (lambda e: e.indirect_dma_start(
                out=Vg[s_], out_offset=None, in_=vbf[:, :], in_offset=bass.IndirectOffsetOnAxis(ap=eT[:, l:l + 1], axis=0))))(),
                reads=['eT', 'vbf'], writes=['Vg%d' % s_], sem='vg%d' % s_)
            sel = IDB[:, l:l + 1].to_broadcast([128, 128])
            for q in range(4):
                h.mm(PS[4 + q], sel, n2b[:, q * 512:(q + 1) * 512], ['cstb', 'n2b'], [psk(4 + q)])
            for hf in range(2):
                h.stt(junk[hf], psall[:, 2048 + hf * 1024:2048 + (hf + 1) * 1024], 1.0, Ug[s_][:, hf * 1024:(hf + 1) * 1024], ALU.mult, ALU.mult,
                      [psk(4 + 2 * hf), psk(5 + 2 * hf), 'Ug%d' % s_], ['junk%d' % hf, 'accs%d' % hf], accum_out=accs[:, hf:hf + 1])
            h.act(accs[:, 2:3], accs[:, 0:1], AF.Gelu, ['accs0', 'accs1'], ['gl'], bias=accs[:, 1:2])
            h.ts('dve', Wl[s_], Zc[:, 127 - l:255 - l], accs[:, 2:3], ALU.mult, ['Zc', 'gl', 'gTt'], ['Wl%d' % s_], s2=gTt[:, l:l + 1], op1=ALU.mult)
            for ct in range(4):
                h.mm(PS[ct], Wl[s_], Vg[s_][:, ct * 512:(ct + 1) * 512], ['Wl%d' % s_, 'Vg%d' % s_], [psk(ct)],
                     start=(l == 0), stop=(l == 127), skip_same=(l > 0))
        if B_LEVEL < 6:
            continue
        for ct in range(4):
            cs4 = slice(ct * 512, (ct + 1) * 512)
            bk = ct % 2
            h.cp('act', tmpb[bk], PS[ct], [psk(ct)], ['tmpb%d' % bk])
            h.tt('dve', tmpb[bk], tmpb[bk], g2b[:, cs4], ALU.mult, ['tmpb%d' % bk, 'bcB5'], ['tmpb%d' % bk])
            h.tt('pool', hb[:, cs4], hb[:, cs4], tmpb[bk], ALU.add, ['hb', 'tmpb%d' % bk], ['hb'])
        if B_LEVEL < 6.1:
            continue
        h.act(outb, hb, AF.Square, ['hb'], ['outb', 'sm'], accum_out=sm[:, 4:5])
        h.act(sm[:, 5:6], sm[:, 4:5], AF.Sqrt, ['sm'], ['sm'], scale=1.0 / D, bias=1e-6)
        h.recip(sm[:, 6:7], sm[:, 5:6], ['sm'], ['sm'])
        h.stt(outb, hb, sm[:, 6:7], fnwb, ALU.mult, ALU.mult, ['hb', 'sm', 'fnwb'], ['outb'])
        if B_LEVEL < 6.2:
            continue
        p.dma('act', out[ob * 128:(ob + 1) * 128, :], outb, reads=['outb'], writes=['out'], sem='outb')
```
